# Optimizing a Trainium2 kernel written in Bass

```python
import math
import jax, jax.numpy as jnp
from jax import lax
import numpy as np

D_MODEL = 2048
BATCH = 2
SEQ = 8192
DEPTH = 4

N_A = DEPTH // 2
N_B = DEPTH - N_A
N_HEADS = 16
HEAD_DIM = 128
N_KV = 4
GROUP = N_HEADS // N_KV
IDX_HEADS = 16
IDX_DIM = 64
TOPK_MAX = 256
WINDOW = 128
BLOCK = 128
N_BUCKETS = 32
MAX_DIST = 128
D_FF = ((8 * D_MODEL + 3 * 256 - 1) // (3 * 256)) * 256
PLE_DIM = 256
EPS = 1e-6
NEG = -1e30

A_Q = N_HEADS * HEAD_DIM
A_KV = N_KV * HEAD_DIM
A_QI = IDX_HEADS * IDX_DIM
A_IN = A_Q + 2 * A_KV + A_QI + IDX_DIM + IDX_HEADS
A_SPLITS = [A_Q, A_Q + A_KV, A_Q + 2 * A_KV, A_Q + 2 * A_KV + A_QI, A_Q + 2 * A_KV + A_QI + IDX_DIM]

kernel_name = "yoco_dsa_swa_sink_hybrid"


def rmsnorm(x, g):
    xf = x.astype(jnp.float32)
    y = xf * lax.rsqrt(jnp.mean(xf * xf, axis=-1, keepdims=True) + EPS)
    return (y * g.astype(jnp.float32)).astype(x.dtype)


def t5_bucket(rel):
    n = jnp.maximum(rel, 0)
    max_exact = N_BUCKETS // 2
    nf = jnp.maximum(n, 1).astype(jnp.float32)
    large = max_exact + (jnp.log(nf / max_exact) / math.log(MAX_DIST / max_exact)
                         * (N_BUCKETS - max_exact)).astype(jnp.int32)
    large = jnp.minimum(large, N_BUCKETS - 1)
    return jnp.where(n < max_exact, n, large)


def swiglu(h, w1, w3, w2):
    return (jax.nn.silu(h @ w1) * (h @ w3)) @ w2


def dsa_attention(h, w_in, w_out, rel_bias):
    B, S, _ = h.shape
    nb = S // BLOCK
    topk = min(TOPK_MAX, S // 4)
    proj = h @ w_in
    q, k, v, qi, ki, wi = jnp.split(proj, A_SPLITS, axis=-1)
    q = q.reshape(B, nb, BLOCK, N_KV, GROUP, HEAD_DIM).swapaxes(0, 1)
    k = k.reshape(B, S, N_KV, HEAD_DIM)
    v = v.reshape(B, S, N_KV, HEAD_DIM)
    qi = qi.reshape(B, nb, BLOCK, IDX_HEADS, IDX_DIM).swapaxes(0, 1)
    wi = wi.reshape(B, nb, BLOCK, IDX_HEADS).swapaxes(0, 1)
    key_pos = jnp.arange(S)

    def block_fn(args):
        j, qb, qib, wib = args
        t = j * BLOCK + jnp.arange(BLOCK)
        sc = jnp.einsum('bqhd,bsd->bqhs', qib, ki).astype(jnp.float32) * (IDX_DIM ** -0.5)
        w = wib.astype(jnp.float32) * (IDX_HEADS ** -0.5)
        score = jnp.einsum('bqhs,bqh->bqs', jax.nn.relu(sc), w)
        causal = key_pos[None, :] <= t[:, None]
        score = jnp.where(causal[None], score, NEG)
        _, idx = lax.top_k(score, topk)
        ks = jax.vmap(lambda kb, ib: kb[ib])(k, idx)
        vs = jax.vmap(lambda vb, ib: vb[ib])(v, idx)
        logits = jnp.einsum('bqgrd,bqkgd->bqgrk', qb, ks).astype(jnp.float32) * (HEAD_DIM ** -0.5)
        rel = t[None, :, None] - idx
        bias = rel_bias[t5_bucket(rel)].astype(jnp.float32)
        bias = bias.reshape(B, BLOCK, topk, N_KV, GROUP).transpose(0, 1, 3, 4, 2)
        valid = (rel >= 0)[:, :, None, None, :]
        logits = jnp.where(valid, logits + bias, NEG)
        probs = jax.nn.softmax(logits, axis=-1).astype(vs.dtype)
        return jnp.einsum('bqgrk,bqkgd->bqgrd', probs, vs)

    out = lax.map(block_fn, (jnp.arange(nb), q, qi, wi))
    out = out.swapaxes(0, 1).reshape(B, S, A_Q)
    return out @ w_out


def band_blocks(a):
    B, S = a.shape[0], a.shape[1]
    ab = a.reshape(B, S // BLOCK, BLOCK, N_KV, HEAD_DIM)
    prev = jnp.pad(ab[:, :-1], ((0, 0), (1, 0), (0, 0), (0, 0), (0, 0)))
    return jnp.concatenate([prev, ab], axis=2)


def swa_attention(h, w_q, w_out, sinks, k_band, v_band, bias_band, valid):
    B, S, _ = h.shape
    nb = S // BLOCK
    q = (h @ w_q).reshape(B, nb, BLOCK, N_KV, GROUP, HEAD_DIM)
    logits = jnp.einsum('bnqgrd,bnkgd->bngrqk', q, k_band).astype(jnp.float32) * (HEAD_DIM ** -0.5)
    logits = jnp.where(valid[None, :, None, None], logits + bias_band[None, None], NEG)
    sink = jnp.broadcast_to(sinks.astype(jnp.float32).reshape(N_KV, GROUP, 1, 1), logits.shape[:-1] + (1,))
    probs = jax.nn.softmax(jnp.concatenate([logits, sink], axis=-1), axis=-1)[..., :-1]
    out = jnp.einsum('bngrqk,bnkgd->bnqgrd', probs.astype(v_band.dtype), v_band)
    return out.reshape(B, S, A_Q) @ w_out


def setup_inputs(seed: int = 0) -> dict:
    key = jax.random.key(seed)
    ks = jax.random.split(key, 20)
    f32 = jnp.float32

    def w(k, shape, fan_in):
        return jax.random.normal(k, shape, f32) * (fan_in ** -0.5)

    def gain(k, shape):
        return 1.0 + 0.02 * jax.random.normal(k, shape, f32)

    return {
        "x": jax.random.normal(ks[0], (BATCH, SEQ, D_MODEL), f32),
        "p": jax.random.normal(ks[1], (DEPTH, BATCH, SEQ, PLE_DIM), f32),
        "w_in_a": w(ks[2], (N_A, D_MODEL, A_IN), D_MODEL),
        "w_out_a": w(ks[3], (N_A, A_Q, D_MODEL), A_Q),
        "w_q_b": w(ks[4], (N_B, D_MODEL, A_Q), D_MODEL),
        "w_out_b": w(ks[5], (N_B, A_Q, D_MODEL), A_Q),
        "sinks": jax.random.normal(ks[6], (N_B, N_HEADS), f32),
        "g_kv": gain(ks[7], (D_MODEL,)),
        "w_kv": w(ks[8], (D_MODEL, 2 * A_KV), D_MODEL),
        "rel_bias": 0.5 * jax.random.normal(ks[9], (N_BUCKETS, N_HEADS), f32),
        "g_attn": gain(ks[10], (DEPTH, D_MODEL)),
        "g_ffn": gain(ks[11], (DEPTH, D_MODEL)),
        "w1": w(ks[12], (DEPTH, D_MODEL, D_FF), D_MODEL),
        "w3": w(ks[13], (DEPTH, D_MODEL, D_FF), D_MODEL),
        "w2": w(ks[14], (DEPTH, D_FF, D_MODEL), D_FF),
        "g_pe": gain(ks[15], (DEPTH, D_MODEL)),
        "w_pe": w(ks[16], (DEPTH, PLE_DIM, D_MODEL), PLE_DIM),
        "w_pg": w(ks[17], (DEPTH, D_MODEL, D_MODEL), D_MODEL),
        "g_final": gain(ks[18], (D_MODEL,)),
    }


def reference(x, p, w_in_a, w_out_a, w_q_b, w_out_b, sinks, g_kv, w_kv, rel_bias,
              g_attn, g_ffn, w1, w3, w2, g_pe, w_pe, w_pg, g_final):
    B, S, _ = x.shape
    nb = S // BLOCK
    qi_ = jnp.arange(BLOCK)
    ci_ = jnp.arange(2 * BLOCK)
    rel = qi_[:, None] + BLOCK - ci_[None, :]
    in_win = (rel >= 0) & (rel < WINDOW)
    key_abs = jnp.arange(nb)[:, None, None] * BLOCK - BLOCK + ci_[None, None, :]
    valid = in_win[None] & (key_abs >= 0)
    bias_band = rel_bias[t5_bucket(rel)].astype(jnp.float32)
    bias_band = bias_band.reshape(BLOCK, 2 * BLOCK, N_KV, GROUP).transpose(2, 3, 0, 1)

    k_band = None
    v_band = None
    for i in range(DEPTH):
        h = rmsnorm(x, g_attn[i])
        if i < N_A:
            x = x + dsa_attention(h, w_in_a[i], w_out_a[i], rel_bias)
        else:
            j = i - N_A
            x = x + swa_attention(h, w_q_b[j], w_out_b[j], sinks[j], k_band, v_band, bias_band, valid)
        x = x + swiglu(rmsnorm(x, g_ffn[i]), w1[i], w3[i], w2[i])
        gate = jax.nn.sigmoid(rmsnorm(x, g_pe[i]) @ w_pg[i])
        x = x + (p[i] @ w_pe[i]) * gate
        if i == N_A - 1:
            kv = rmsnorm(x, g_kv) @ w_kv
            k_s, v_s = jnp.split(kv, 2, axis=-1)
            k_band = band_blocks(k_s.reshape(B, S, N_KV, HEAD_DIM))
            v_band = band_blocks(v_s.reshape(B, S, N_KV, HEAD_DIM))
    return rmsnorm(x, g_final)
```

```python
import contextlib
import math
import numpy as np
import concourse.bass as bass
import concourse.mybir as mybir
from concourse.bass_utils import run_bass_kernel_spmd

F32 = mybir.dt.float32
BF16 = mybir.dt.bfloat16
ALU = mybir.AluOpType
AF = mybir.ActivationFunctionType
AX = mybir.AxisListType

D = 2048
T = 2048
DFF = 5632
NEG = -1.0e30
MNEG = -30000.0
EPS = 1e-6
XL = 8192 + 8192 + 2048
NBIS = 26
NDS = 12


class Sched:
    def __init__(self, nc, es):
        self.nc = nc
        self.engs = {'pe': nc.tensor, 'act': nc.scalar, 'dve': nc.vector, 'pool': nc.gpsimd, 'sp': nc.sync}
        self.sem = {e: es.enter_context(nc.semaphore('s_' + e)) for e in ('pe', 'act', 'dve', 'pool')}
        self.cnt = {e: 0 for e in self.sem}
        self.seen = {e: {} for e in self.engs}
        self.res = {}
        self.dsems = [es.enter_context(nc.semaphore('d%d' % i)) for i in range(NDS)]
        self.dval = [0] * NDS
        self.dnext = 0
        self.ccsem = es.enter_context(nc.semaphore('s_cc'))
        self.ccval = 0

    def _wait(self, e, tok):
        name, sem, val, src = tok
        if self.seen[e].get(name, 0) >= val:
            return
        self.engs[e].wait_ge(sem, val)
        self.seen[e][name] = val

    def _deps(self, e, reads, writes, is_dma):
        for k in reads:
            r = self.res.get(k)
            if r and r[0] is not None:
                self._dep1(e, r[0], 'raw', is_dma)
        for k in writes:
            r = self.res.get(k)
            if r:
                if r[0] is not None:
                    self._dep1(e, r[0], 'waw', is_dma)
                for t in r[1].values():
                    self._dep1(e, t, 'war', is_dma)

    def _dep1(self, e, tok, kind, is_dma):
        if (not is_dma) and tok[3] == e:
            if e == 'pe':
                return
        self._wait(e, tok)

    def _commit(self, tok, reads, writes):
        for k in reads:
            r = self.res.setdefault(k, [None, {}])
            r[1][tok[0]] = tok
        for k in writes:
            self.res[k] = [tok, {}]

    def op(self, e, reads, writes, fn):
        self._deps(e, reads, writes, False)
        ins = fn(self.engs[e])
        self.cnt[e] += 1
        ins.then_inc(self.sem[e], 1)
        self._commit((e, self.sem[e], self.cnt[e], e), reads, writes)

    def dma(self, out, in_, reads, writes, q='sp'):
        self._deps(q, reads, writes, True)
        i = self.dnext
        self.dnext = (i + 1) % NDS
        name = 'd%d' % i
        if self.dval[i] > 0:
            self._wait(q, (name, self.dsems[i], self.dval[i], 'dma'))
        self.dval[i] += 16
        self.engs[q].dma_start(out=out, in_=in_).then_inc(self.dsems[i], 16)
        self._commit((name, self.dsems[i], self.dval[i], 'dma'), reads, writes)

    def barrier(self):
        for e in self.engs:
            for x in self.sem:
                if x != e and self.cnt[x] > 0:
                    self._wait(e, (x, self.sem[x], self.cnt[x], x))
            for i in range(NDS):
                if self.dval[i] > 0:
                    self._wait(e, ('d%d' % i, self.dsems[i], self.dval[i], 'dma'))
        self.res = {}

    def finish(self):
        for i in range(NDS):
            if self.dval[i] > 0:
                self._wait('sp', ('d%d' % i, self.dsems[i], self.dval[i], 'dma'))


class Prog:
    def __init__(self, ext_in, ext_out):
        self.nc = bass.Bass("TRN2", target_bir_lowering=False)
        self.ext_in = set(ext_in)
        self.ext_out = set(ext_out)
        self.D = {}
        self.uid = 0

    def dram(self, name, shape, dt):
        if name in self.D:
            return self.D[name]
        kind = "ExternalInput" if name in self.ext_in else ("ExternalOutput" if name in self.ext_out else "Internal")
        ap = self.nc.dram_tensor(name, list(shape), dt, kind=kind).ap()
        self.D[name] = ap
        return ap

    def sb(self, st, name, shape, dt):
        self.uid += 1
        return st.enter_context(self.nc.sbuf_tensor("%s_%d" % (name, self.uid), list(shape), dt))

    def begin(self, es):
        nc = self.nc
        self.es = es
        self.S = Sched(nc, es)
        S = self.S
        self.ps = es.enter_context(nc.psum_tensor("ps", [128, 8, 512], F32))
        d = self.dram
        d('ident', [128, 128], F32); d('gains', [128, 14 * 16], F32)
        d('cmask', [128, 512], F32); d('swamask', [128, 640], F32)
        d('cb', [128, 16], F32); d('biasg', [128, 16 * 640], F32); d('sinks', [128, 32], F32)
        self.identf = self.sb(es, 'identf', [128, 128], F32)
        self.ident = self.sb(es, 'ident', [128, 128], BF16)
        self.I4 = self.sb(es, 'I4', [128, 4, 128], BF16)
        self.ones = self.sb(es, 'ones', [128, 128], BF16)
        self.gains = self.sb(es, 'gains', [128, 14 * 16], F32)
        S.dma(self.identf[:], self.D['ident'][:, :], [], ['identf'])
        S.dma(self.gains[:], self.D['gains'][:, :], [], ['gains'])
        S.op('dve', ['identf'], ['ident'], lambda v: v.tensor_copy(out=self.ident[:], in_=self.identf[:]))
        for k in range(4):
            S.op('dve', ['identf'], ['I4'], lambda v, k=k: v.tensor_copy(out=self.I4[:, k, :], in_=self.identf[:]))
        S.op('dve', [], ['ones'], lambda v: v.memset(self.ones[:], 1.0))
        with contextlib.ExitStack() as zs:
            zt = self.sb(zs, 'zt', [128, 2048], BF16)
            S.op('dve', [], ['zt'], lambda v: v.memset(zt[:], 0.0))
            S.dma(self.Lc(8)[64:128, :], zt[64:128, :], ['zt'], [('L', 'kipad')])
            S.barrier()
        self.epsb = self.sb(es, 'epsb', [128, 1], F32)
        S.op('dve', [], ['epsb'], lambda v: v.memset(self.epsb[:], EPS))
        S.barrier()

    def Lc(self, k):
        return self.dram('L%d' % k, [128, 2048], BF16)

    def G3c(self, gname, k):
        return self.dram('%s_%d' % (gname, k), [512, 2048], BF16).rearrange("(r p) x -> p r x", p=128)

    def bank(self, k, n=512):
        return self.ps[:, k, 0:n]

    def load_w(self, st, Wsl, KC, nw, dst, dst_key, stg, stg_keys, slot, cast='pool'):
        S = self.S
        sview = stg[slot][:, 0:KC, 0:nw]
        Wr = Wsl.rearrange("(kc p) n -> p kc n", p=128)
        keys = []
        for k0 in range(0, KC, 16):
            k1 = min(KC, k0 + 16)
            key = (stg_keys[slot], k0)
            keys.append(key)
            S.dma(stg[slot][:, k0:k1, 0:nw], Wr[:, k0:k1, :], [], [key])
        if cast == 'act':
            S.op('act', keys, [dst_key], lambda a: a.activation(out=dst, in_=sview, func=AF.Copy))
        else:
            S.op(cast, keys, [dst_key], lambda v: v.tensor_copy(out=dst, in_=sview))

    def norm(self, st, xname, gi, hT, t0, tn, hkey, TC=512, final=None):
        S, nc = self.S, self.nc
        xT = self.D[xname]
        with contextlib.ExitStack() as ph:
            xt2 = [self.sb(ph, 'nx%d' % k, [128, 16, TC], F32) for k in range(2)]
            sq = [self.sb(ph, 'nsq%d' % k, [128, TC], BF16) for k in range(3)]
            sd = self.sb(ph, 'nsd', [128, TC], F32)
            rs = self.sb(ph, 'nrs', [128, TC], F32)
            ot = [self.sb(ph, 'not%d' % k, [128, TC], F32) for k in range(2)] if final else None
            B = 7
            for tc in range(tn // TC):
                ta = t0 + tc * TC
                xt = xt2[tc % 2]
                xs_ = tc % 2
                for c in range(16):
                    S.dma(xt[:, c, :], xT[c, :, ta:ta + TC], [(xname, c, ta // 512)], [('nx', xs_, c)])
                for c in range(16):
                    k = c % 3
                    S.op('act', [('nx', xs_, c)], [('nsq', k)],
                         lambda a, c=c, k=k, xt=xt: a.activation(out=sq[k][:], in_=xt[:, c, :], func=AF.Square))
                    S.op('pe', [('nsq', k), 'ones'], [('ps', B)],
                         lambda p, c=c, k=k: p.matmul(self.bank(B, TC), lhsT=self.ones[:], rhs=sq[k][:],
                                                      start=(c == 0), stop=(c == 15)))
                S.op('act', [('ps', B)], ['nsd'],
                     lambda a: a.activation(out=sd[:], in_=self.bank(B, TC), func=AF.Sqrt, bias=self.epsb[:, 0:1], scale=1.0 / D))
                S.op('dve', ['nsd'], ['nrs'], lambda v: v.reciprocal(out=rs[:], in_=sd[:]))
                for c in range(16):
                    gcol = self.gains[:, gi * 16 + c: gi * 16 + c + 1]
                    if final is None:
                        S.op('dve', [('nx', xs_, c), 'nrs', 'gains'], [hkey],
                             lambda v, c=c, gcol=gcol, xt=xt, tc=tc: v.scalar_tensor_tensor(
                                 out=hT[:, c, tc * TC:(tc + 1) * TC], in0=xt[:, c, :], scalar=gcol, in1=rs[:],
                                 op0=ALU.mult, op1=ALU.mult))
                    else:
                        k = c % 2
                        S.op('dve', [('nx', xs_, c), 'nrs', 'gains'], [('not', k)],
                             lambda v, c=c, gcol=gcol, k=k, xt=xt: v.scalar_tensor_tensor(
                                 out=ot[k][:], in0=xt[:, c, :], scalar=gcol, in1=rs[:],
                                 op0=ALU.mult, op1=ALU.mult))
                        S.dma(self.D[final][c, :, ta:ta + TC], ot[k][:], [('not', k)], [(final, c, ta // 512)])
            S.barrier()

    def linear_T(self, st, Wap, K, slabs, NW, AT, at_keys, ntc, epilogue, banks, mats=1, W2ap=None, K2=None, AT2=None,
                 cast=('pool',)):
        S = self.S
        KC = K // 128
        KC2 = (K2 // 128) if K2 else KC
        stg = [self.sb(st, 'wst%d' % k, [128, KC, NW], F32) for k in range(2)]
        wbf = [self.sb(st, 'wbf%d' % k, [128, KC, NW], BF16) for k in range(2)]
        skeys = [('wst', 0), ('wst', 1)]
        if mats == 2:
            stg2 = [self.sb(st, 'wsu%d' % k, [128, KC2, NW], F32) for k in range(2)]
            wbf2 = [self.sb(st, 'wbu%d' % k, [128, KC2, NW], BF16) for k in range(2)]
            skeys2 = [('wsu', 0), ('wsu', 1)]
        if AT2 is None:
            AT2 = AT
        ci = [0]

        def load(si):
            c0, nw = slabs[si]
            sl = si % 2
            ce = cast[ci[0] % len(cast)]; ci[0] += 1
            self.load_w(st, Wap[:, c0:c0 + nw], KC, nw, wbf[sl][:, :, 0:nw], ('wbf', sl), stg, skeys, sl, ce)
            if mats == 2:
                ce = cast[ci[0] % len(cast)]; ci[0] += 1
                self.load_w(st, W2ap[:, c0:c0 + nw], KC2, nw, wbf2[sl][:, :, 0:nw], ('wbu', sl), stg2, skeys2, sl, ce)

        load(0)
        bi = 0
        for si, (c0, nw) in enumerate(slabs):
            if si + 1 < len(slabs):
                load(si + 1)
            sl = si % 2
            for n0 in range(0, nw, 128):
                nn = min(128, nw - n0)
                for tc in range(ntc):
                    b1 = banks[bi % len(banks)]; bi += 1

                    def mm(p, b=b1, w=wbf[sl], kc_n=KC, A=AT, n0=n0, nn=nn, tc=tc):
                        ins = None
                        for kc in range(kc_n):
                            ins = p.matmul(self.ps[0:nn, b, :], lhsT=w[:, kc, n0:n0 + nn], rhs=A[:, kc, tc * 512:(tc + 1) * 512],
                                           start=(kc == 0), stop=(kc == kc_n - 1))
                        return ins
                    S.op('pe', [('wbf', sl)] + at_keys, [('ps', b1)], mm)
                    b2 = None
                    if mats == 2:
                        b2 = banks[bi % len(banks)]; bi += 1

                        def mm2(p, b=b2, w=wbf2[sl], kc_n=KC2, A=AT2, n0=n0, nn=nn, tc=tc):
                            ins = None
                            for kc in range(kc_n):
                                ins = p.matmul(self.ps[0:nn, b, :], lhsT=w[:, kc, n0:n0 + nn], rhs=A[:, kc, tc * 512:(tc + 1) * 512],
                                               start=(kc == 0), stop=(kc == kc_n - 1))
                            return ins
                        S.op('pe', [('wbu', sl)] + at_keys, [('ps', b2)], mm2)
                    epilogue(b1, b2, c0 + n0, nn, tc)

    def make_resid(self, st, xsrc, xdst, tbase=0):
        S = self.S
        xr = [self.sb(st, 'xr%d' % k, [128, 512], F32) for k in range(3)]
        cnt = [0]

        def epi(val_ap, val_keys, c, tcg, eng='dve'):
            k = cnt[0] % 3; cnt[0] += 1
            S.dma(xr[k][:], self.D[xsrc][c, :, tcg * 512:(tcg + 1) * 512], [(xsrc, c, tcg)], [('xr', k)])
            S.op(eng, [('xr', k)] + val_keys, [('xr', k)],
                 lambda v: v.tensor_tensor(out=xr[k][:], in0=val_ap, in1=xr[k][:], op=ALU.add))
            S.dma(self.D[xdst][c, :, tcg * 512:(tcg + 1) * 512], xr[k][:], [('xr', k)], [(xdst, c, tcg)])
        return epi

    def kv_proj(self, st, Wap, kcol, vcol, hT, evac):
        S = self.S
        def epi_k(b1, b2, col, nn, tc):
            g = (col - kcol) // 128
            evac(b1, nn, self.Lc(g)[0:nn, tc * 512:(tc + 1) * 512], [('L', 'k', g, tc)])
        self.linear_T(st, Wap, D, [(kcol, 256), (kcol + 256, 256)], 256, hT, ['hT'], 4, epi_k, [0, 1, 2, 3])
        S.barrier()

    def v_proj(self, st, Wap, vcol, hT, ncol, dst_fn, out_dt, name):
        S = self.S
        with contextlib.ExitStack() as ph:
            stg = [self.sb(ph, 'vst', [128, 16, 256], F32)]
            wv = self.sb(ph, 'wv', [128, 16, ncol], BF16)
            for c0 in range(0, ncol, 256):
                nw = min(256, ncol - c0)
                self.load_w(ph, Wap[:, vcol + c0: vcol + c0 + nw], 16, nw, wv[:, :, c0:c0 + nw], ('wv', c0), stg, [('vst', 0)], 0, 'dve')
            ot = [self.sb(ph, 'vo%d' % k, [128, ncol], out_dt) for k in range(3)]
            wkeys = [('wv', c0) for c0 in range(0, ncol, 256)]
            for tt in range(16):
                b = tt % 4
                k = tt % 3

                def mm(p, b=b, tt=tt):
                    ins = None
                    for kc in range(16):
                        ins = p.matmul(self.ps[:, b, 0:ncol], lhsT=hT[:, kc, tt * 128:(tt + 1) * 128], rhs=wv[:, kc, :],
                                       start=(kc == 0), stop=(kc == 15))
                    return ins
                S.op('pe', wkeys + ['hT'], [('ps', b)], mm)
                S.op('act', [('ps', b)], [('vo', k)], lambda a, b=b, k=k: a.activation(out=ot[k][:], in_=self.ps[:, b, 0:ncol], func=AF.Copy))
                if ncol == 512:
                    for g in range(4):
                        S.dma(self.Lc(4 + g)[:, tt * 128:(tt + 1) * 128], ot[k][:, g * 128:(g + 1) * 128], [('vo', k)], [('L', 'v', g, tt)])
                else:
                    dst, dkeys = dst_fn(tt)
                    S.dma(dst, ot[k][:], [('vo', k)], dkeys)
            S.barrier()

    def make_evac(self, st):
        S = self.S
        ev = [self.sb(st, 'ev%d' % k, [128, 512], BF16) for k in range(4)]
        cnt = [0]

        def evac(b, nn, dst, dkeys, mul=1.0):
            k = cnt[0] % 4; cnt[0] += 1
            if cnt[0] % 2:
                S.op('act', [('ps', b)], [('ev', k)], lambda a: a.activation(out=ev[k][0:nn, :], in_=self.ps[0:nn, b, :], func=AF.Copy, scale=mul))
            else:
                S.op('dve', [('ps', b)], [('ev', k)], lambda v: v.tensor_scalar(out=ev[k][0:nn, :], in0=self.ps[0:nn, b, :], scalar1=mul, scalar2=None, op0=ALU.mult))
            S.dma(dst, ev[k][0:nn, :], [('ev', k)], dkeys)
        return evac

    def phase_A(self, xname, l):
        S = self.S
        d = self.dram
        d('qT', [16, 128, T], BF16); d('qiT', [8, 128, T], BF16); d('wi', [T, 16], F32)
        Wap = self.D['w_in_a_%d' % l]
        with contextlib.ExitStack() as st:
            hT = self.sb(st, 'hT', [128, 16, T], BF16)
            self.norm(st, xname, 0 + l, hT, 0, T, 'hT')
            with contextlib.ExitStack() as ph:
                evac = self.make_evac(ph)
                qT, qiT = self.D['qT'], self.D['qiT']

                def epi(b1, b2, col, nn, tc):
                    ts = slice(tc * 512, (tc + 1) * 512)
                    if col < 2048:
                        evac(b1, nn, qT[col // 128, :, ts], [('qT', col // 128, tc)], mul=128.0 ** -0.5)
                    elif col < 2560:
                        g = (col - 2048) // 128
                        evac(b1, nn, self.Lc(g)[:, tc * 512:(tc + 1) * 512], [('L', 'k', g, tc)])
                    elif col < 4096:
                        pr = (col - 3072) // 128
                        evac(b1, nn, qiT[pr, :, ts], [('qiT', pr, tc)])
                    else:
                        evac(b1, nn, self.Lc(8)[0:64, tc * 512:(tc + 1) * 512], [('L', 'ki', tc)])
                slabs = [(c, 256) for c in range(0, 2560, 256)] + [(c, 256) for c in range(3072, 4096, 256)] + [(4096, 64)]
                self.linear_T(ph, Wap, D, slabs, 256, hT, ['hT'], 4, epi, [0, 1, 2, 3])
                S.barrier()
            self.v_proj(st, Wap, 2560, hT, 512, None, BF16, 'v')
            self.v_proj(st, Wap, 4160, hT, 16, lambda tt: (self.D['wi'][tt * 128:(tt + 1) * 128, :], [('wi', tt)]), F32, 'wi')

    def phase_KV(self, xname):
        S = self.S
        Wap = self.D['w_kv']
        with contextlib.ExitStack() as st:
            hT = self.sb(st, 'hT', [128, 16, T], BF16)
            self.norm(st, xname, 12, hT, 0, T, 'hT')
            with contextlib.ExitStack() as ph:
                evac = self.make_evac(ph)
                self.kv_proj(ph, Wap, 0, 512, hT, evac)
            self.v_proj(st, Wap, 512, hT, 512, None, BF16, 'v')

    def bias_tables(self, st, dsa):
        S = self.S
        tb = self.sb(st, 'biastb', [128, 16, 640], BF16)
        with contextlib.ExitStack() as ph:
            bg = self.sb(ph, 'bg', [128, 16, 640], F32)
            aux = self.sb(ph, 'aux', [128, 640], F32)
            S.dma(bg[:], self.D['biasg'].rearrange("p (h x) -> p h x", h=16), [], ['bg'])
            if dsa:
                S.dma(aux[:, 0:16], self.D['cb'][:, :], [], ['aux'])
                for h in range(16):
                    S.op('dve', ['bg', 'aux'], ['biastb'],
                         lambda v, h=h: v.tensor_scalar(out=tb[:, h, :], in0=bg[:, h, :], scalar1=aux[:, h:h + 1], scalar2=None, op0=ALU.subtract))
            else:
                S.dma(aux[:], self.D['swamask'][:, :], [], ['aux'])
                for h in range(16):
                    S.op('dve', ['bg', 'aux'], ['biastb'],
                         lambda v, h=h: v.tensor_tensor(out=tb[:, h, :], in0=bg[:, h, :], in1=aux[:], op=ALU.add))
            S.barrier()
        return tb

    def phase_B(self, gname='G'):
        S, nc = self.S, self.nc
        d = self.dram
        qT = d('qT', [16, 128, T], BF16); qiT = d('qiT', [8, 128, T], BF16); wiD = d('wi', [T, 16], F32)
        aT = d('attnT', [16, 128, T], BF16)
        with contextlib.ExitStack() as st:
            tb = self.bias_tables(st, True)
            kiT = self.sb(st, 'kiT', [128, 4, 2048], BF16)
            score = self.sb(st, 'score', [128, 4, 2048], F32)
            nmask2 = [self.sb(st, 'nmask%d' % k, [128, 4, 2048], BF16) for k in range(2)]
            cmask = self.sb(st, 'cmask', [128, 4, 128], F32)
            kT = [self.sb(st, 'kT%d' % k, [128, 4, 2048], BF16) for k in range(2)]
            Vs = [self.sb(st, 'Vs%d' % k, [128, 4, 2048], BF16) for k in range(2)]
            qs = [self.sb(st, 'qs%d' % k, [128, 16, 128], BF16) for k in range(2)]
            qis = [self.sb(st, 'qis%d' % k, [128, 8, 128], BF16) for k in range(2)]
            wis = [self.sb(st, 'wis%d' % k, [128, 16], F32) for k in range(2)]
            diag = [self.sb(st, 'diag%d' % k, [128, 16, 128], BF16) for k in range(1)] * 2
            Rt = [self.sb(st, 'R%d' % k, [128, 512], BF16) for k in range(3)]
            PT = [self.sb(st, 'PT%d' % k, [128, 512], BF16) for k in range(3)]
            Mg = [self.sb(st, 'Mg%d' % k, [128, 4, 5, 128], BF16) for k in range(1)] * 2
            osb = [self.sb(st, 'osb%d' % k, [128, 512], F32) for k in range(2)]
            rd = [self.sb(st, 'rd%d' % k, [128, 512], F32) for k in range(2)]
            ao = [self.sb(st, 'ao%d' % k, [128, 512], BF16) for k in range(2)]
            bs = self.sb(st, 'bs', [128, 8], F32)
            cn = [self.sb(st, 'cn%d' % k, [128, 32], F32) for k in range(2)]
            tau = [self.sb(st, 'tau%d' % k, [128, 1], F32) for k in range(2)]
            S.dma(cmask[:], self.D['cmask'].rearrange("p (r s) -> p r s", r=4), [], ['cmask'])
            S.dma(kiT[0:64, :, :], self.G3c(gname, 8)[0:64, :, :], [], ['kiT0'])
            S.dma(kiT[64:128, :, :], self.G3c(gname, 8)[0:64, :, :], [], ['kiT1'])

            cntR = [0]; cntP = [0]
            SB = [0, 1, 2]
            sbi = [0]

            def indexer(i):
                sl = i % 2
                seg = (i + 1) * 128
                S.dma(qs[sl][:], qT[:, :, i * 128:(i + 1) * 128].rearrange("h d t -> d h t"), [('qT', h, i // 4) for h in range(16)], [('qs', sl)])
                S.dma(qis[sl][:], qiT[:, :, i * 128:(i + 1) * 128].rearrange("h d t -> d h t"), [('qiT', h, i // 4) for h in range(8)], [('qis', sl)])
                S.dma(wis[sl][:], wiD[i * 128:(i + 1) * 128, :], [('wi', i)], [('wis', sl)])
                for h in range(16):
                    S.op('pool', [('wis', sl), 'ident'], ['diag'],
                         lambda v, h=h: v.tensor_scalar(out=diag[sl][:, h, :], in0=self.ident[:], scalar1=wis[sl][:, h:h + 1], scalar2=None, op0=ALU.mult))
                pieces = []
                for r in range(4):
                    c0 = 0
                    while c0 < seg:
                        n = min(512, seg - c0)
                        pieces.append((r, c0, n)); c0 += n
                for (r, c0, n) in pieces:
                    LA = 2
                    pend = []

                    def emitS(h):
                        b = SB[sbi[0] % 3]; sbi[0] += 1
                        po = (h % 2) * 64
                        S.op('pe', [('qis', sl), 'kiT0', 'kiT1'], [('ps', b)],
                             lambda p: p.matmul(self.ps[:, b, 0:n], lhsT=qis[sl][po:po + 64, h // 2, :], rhs=kiT[po:po + 64, r, c0:c0 + n],
                                                start=True, stop=True))
                        k = cntR[0] % 3; cntR[0] += 1
                        S.op('act', [('ps', b)], [('R', k)],
                             lambda a: a.activation(out=Rt[k][:, 0:n], in_=self.ps[:, b, 0:n], func=AF.Relu, scale=1.0 / 32.0))
                        return k

                    def emitD(h, k):
                        S.op('pe', [('R', k), 'diag'], [('ps', 7)],
                             lambda p: p.matmul(self.ps[:, 7, 0:n], lhsT=diag[sl][:, h, :], rhs=Rt[k][:, 0:n], start=(h == 0), stop=(h == 15)))
                    for h in range(16):
                        pend.append((h, emitS(h)))
                        if len(pend) > LA:
                            emitD(*pend.pop(0))
                    while pend:
                        emitD(*pend.pop(0))
                    S.op('dve', [('ps', 7)], ['score'], lambda v: v.tensor_copy(out=score[:, r, c0:c0 + n], in_=self.ps[:, 7, 0:n]))

            def bisect(i):
                sl = i % 2
                seg = (i + 1) * 128
                sc = score[:, :, 0:seg]
                jk = nmask2[sl][:, :, 0:seg]
                nm = jk
                S.op('dve', ['score'], ['bs0'], lambda v: v.tensor_reduce(out=bs[:, 0:1], in_=sc, axis=AX.XY, op=ALU.max))
                S.op('dve', ['score'], ['bs1'], lambda v: v.tensor_reduce(out=bs[:, 1:2], in_=sc, axis=AX.XY, op=ALU.min))
                S.op('dve', ['score', 'cmask'], ['score'],
                     lambda v: v.tensor_tensor(out=score[:, :, i * 128:(i + 1) * 128], in0=score[:, :, i * 128:(i + 1) * 128], in1=cmask[:], op=ALU.add))
                S.op('dve', ['bs0', 'bs1'], ['bs2'], lambda v: v.tensor_tensor(out=bs[:, 2:3], in0=bs[:, 0:1], in1=bs[:, 1:2], op=ALU.subtract))
                S.op('dve', ['bs2'], ['bs2'], lambda v: v.tensor_scalar(out=bs[:, 2:3], in0=bs[:, 2:3], scalar1=1.0001, scalar2=1e-6, op0=ALU.mult, op1=ALU.add))
                S.op('dve', ['bs1'], ['lo'], lambda v: v.tensor_copy(out=tau[sl][:], in_=bs[:, 1:2]))
                S.op('dve', [], [('cn', sl)], lambda v: v.memset(cn[sl][:], 0.0))
                for k in range(NBIS):
                    ck = 2.0 ** -(k + 1)
                    S.op('dve', ['bs2', 'lo'], ['mid'],
                         lambda v: v.scalar_tensor_tensor(out=bs[:, 4:5], in0=bs[:, 2:3], scalar=ck, in1=tau[sl][:], op0=ALU.mult, op1=ALU.add))
                    S.op('dve', ['score', 'mid', ('cn', sl)], [('nmask', sl), ('cn', sl)],
                         lambda v, k=k: v.tensor_scalar(out=jk, in0=sc, scalar1=bs[:, 4:5], scalar2=0.0, op0=ALU.is_ge, op1=ALU.add,
                                                        accum_out=cn[sl][:, k:k + 1]))
                    S.op('dve', [('cn', sl)], ['tmp'],
                         lambda v, k=k: v.tensor_scalar(out=bs[:, 5:6], in0=cn[sl][:, k:k + 1], scalar1=256.0, scalar2=ck, op0=ALU.is_ge, op1=ALU.mult))
                    S.op('dve', ['tmp', 'bs2', 'lo'], ['lo'],
                         lambda v: v.scalar_tensor_tensor(out=tau[sl][:], in0=bs[:, 5:6], scalar=bs[:, 2:3], in1=tau[sl][:], op0=ALU.mult, op1=ALU.add))
                S.op('dve', ['score', 'lo'], [('nmask', sl)],
                     lambda v: v.tensor_scalar(out=nm, in0=sc, scalar1=tau[sl][:], scalar2=MNEG, op0=ALU.is_lt, op1=ALU.mult))

            kvc = [0]

            def load_kv(i, g):
                sl = kvc[0] % 2; kvc[0] += 1
                seg = (i + 1) * 128
                S.dma(kT[sl][:, :, 0:seg], self.G3c(gname, g)[:, :, 0:seg], [], [('kT', sl)])
                S.dma(Vs[sl][:, :, 0:seg], self.G3c(gname, 4 + g)[:, :, 0:seg], [], [('Vs', sl)])
                return sl

            def attention(i, slot0):
                sl = i % 2
                blocks = [(r, ib) for r in range(4) for ib in range(i + 1)]
                near = {(r, i): 1 + r for r in range(4)}
                if i > 0:
                    near[(3, i - 1)] = 0
                kvslots = {0: slot0}
                nxt = None
                for g in range(4):
                    if g + 1 < 4:
                        kvslots[g + 1] = load_kv(i, g + 1)
                    elif i + 1 < 16:
                        nxt = load_kv(i + 1, 0)
                    ks = kvslots[g]
                    ms = 0
                    nmask = nmask2[sl]
                    OB = 3 + (g % 2)
                    DB = 5 + (g % 2)
                    for hh in range(4):
                        S.op('pool', [('nmask', sl), 'biastb'], [('Mg', ms)],
                             lambda v, hh=hh: v.tensor_tensor(out=Mg[ms][:, hh, 1:5, :], in0=tb[:, g * 4 + hh, :].rearrange("p (b s) -> p b s", b=5)[:, 1:5, :],
                                                              in1=nmask[:, :, i * 128:(i + 1) * 128], op=ALU.add))
                        if i > 0:
                            S.op('pool', [('nmask', sl), 'biastb'], [('Mg', ms)],
                                 lambda v, hh=hh: v.tensor_tensor(out=Mg[ms][:, hh, 0, :], in0=tb[:, g * 4 + hh, 0:128],
                                                                  in1=nmask[:, 3, (i - 1) * 128:i * 128], op=ALU.add))
                    LA = 2
                    pend = []

                    def emitS(bi_, r, ib):
                        b = SB[sbi[0] % 3]; sbi[0] += 1

                        def mm(p):
                            p.matmul(self.ps[:, b, :], lhsT=kT[ks][:, r, ib * 128:(ib + 1) * 128], rhs=qs[sl][:, g * 4:(g + 1) * 4, :],
                                     start=True, stop=False)
                            if (r, ib) in near:
                                nb = near[(r, ib)]
                                ins = None
                                for hh in range(4):
                                    ins = p.matmul(self.ps[:, b, hh * 128:(hh + 1) * 128], lhsT=Mg[ms][:, hh, nb, :], rhs=self.ident[:],
                                                   start=False, stop=(hh == 3))
                                return ins
                            return p.matmul(self.ps[:, b, :], lhsT=nmask[:, r, ib * 128:(ib + 1) * 128], rhs=self.I4[:], start=False, stop=True)
                        S.op('pe', [('kT', ks), ('qs', sl), ('nmask', sl), ('Mg', ms), 'ident', 'I4'], [('ps', b)], mm)
                        k = cntP[0] % 3; cntP[0] += 1
                        S.op('act', [('ps', b)], [('PT', k)], lambda a: a.activation(out=PT[k][:], in_=self.ps[:, b, :], func=AF.Exp))
                        return k

                    def emitPV(bi_, r, ib, k):
                        first = (bi_ == 0); last = (bi_ == len(blocks) - 1)

                        def mm(p):
                            p.matmul(self.ps[:, OB, :], lhsT=Vs[ks][:, r, ib * 128:(ib + 1) * 128], rhs=PT[k][:], start=first, stop=last)
                            return p.matmul(self.ps[:, DB, :], lhsT=self.ones[:], rhs=PT[k][:], start=first, stop=last)
                        S.op('pe', [('Vs', ks), ('PT', k), 'ones'], [('ps', OB), ('ps', DB)], mm)
                    for bi_, (r, ib) in enumerate(blocks):
                        pend.append((bi_, r, ib, emitS(bi_, r, ib)))
                        if len(pend) > LA:
                            emitPV(*pend.pop(0))
                    while pend:
                        emitPV(*pend.pop(0))
                    a = g % 2
                    S.op('act', [('ps', DB)], [('rd', a)], lambda a_: a_.activation(out=rd[a][:], in_=self.ps[:, DB, :], func=AF.Ln))
                    S.op('act', [('rd', a)], [('rd', a)], lambda a_: a_.activation(out=rd[a][:], in_=rd[a][:], func=AF.Exp, scale=-1.0))
                    S.op('act', [('ps', OB)], [('osb', a)], lambda a_: a_.activation(out=osb[a][:], in_=self.ps[:, OB, :], func=AF.Copy))
                    S.op('pool', [('osb', a), ('rd', a)], [('ao', a)], lambda v: v.tensor_tensor(out=ao[a][:], in0=osb[a][:], in1=rd[a][:], op=ALU.mult))
                    S.dma(aT[g * 4:(g + 1) * 4, :, i * 128:(i + 1) * 128].rearrange("h d t -> d h t"), ao[a][:].rearrange("p (h t) -> p h t", h=4),
                          [('ao', a)], [('attnT', g * 4 + hh, i // 4) for hh in range(4)])
                return nxt

            indexer(0)
            bisect(0)
            slot0 = load_kv(0, 0)
            for i in range(16):
                if i + 1 < 16:
                    indexer(i + 1)
                    bisect(i + 1)
                slot0 = attention(i, slot0)
            S.barrier()

    def phase_C(self, xsrc, xdst, l, w_out_name, w_out_idx, upto=None):
        S = self.S
        d = self.dram
        aT = d('attnT', [16, 128, T], BF16)
        with contextlib.ExitStack() as st:
            AT = self.sb(st, 'aTs', [128, 16, T], BF16)
            for tc in range(4):
                S.dma(AT[:, :, tc * 512:(tc + 1) * 512], aT[:, :, tc * 512:(tc + 1) * 512].rearrange("h d t -> d h t"),
                      [('attnT', h, tc) for h in range(16)], [('AT', tc)])
            resid = self.make_resid(st, xsrc, xdst)

            def epi(b1, b2, col, nn, tc):
                resid(self.ps[:, b1, :], [('ps', b1)], col // 128, tc)
            self.linear_T(st, self.D['%s_%d' % (w_out_name, w_out_idx)], D, [(c, 256) for c in range(0, D, 256)], 256, AT, [('AT', tc_) for tc_ in range(4)], 4, epi, [0, 1, 2, 3])
            S.barrier()
        if upto == 'C1':
            return
        for half in range(2):
            t0 = half * 1024
            with contextlib.ExitStack() as st:
                gT = self.sb(st, 'gT', [128, 44, 1024], BF16)
                with contextlib.ExitStack() as st2:
                    hT = self.sb(st2, 'hTh', [128, 16, 1024], BF16)
                    self.norm(st2, xdst, 4 + l, hT, t0, 1024, 'hT')
                    with contextlib.ExitStack() as st3:
                        sg = [self.sb(st3, 'sg%d' % k, [128, 512], F32) for k in range(3)]
                        cs = [0]

                        def epi_up(b1, b2, col, nn, tc):
                            k = cs[0] % 3; cs[0] += 1
                            S.op('act', [('ps', b1)], [('sg', k)], lambda a: a.activation(out=sg[k][:], in_=self.ps[:, b1, :], func=AF.Silu))
                            S.op('dve', [('sg', k), ('ps', b2)], ['gT'],
                                 lambda v: v.tensor_tensor(out=gT[:, col // 128, tc * 512:(tc + 1) * 512], in0=sg[k][:], in1=self.ps[:, b2, :], op=ALU.mult))
                        self.linear_T(st3, self.D['w1_%d' % l], D, [(c, 128) for c in range(0, DFF, 128)], 128, hT, ['hT'], 2, epi_up,
                                      [0, 1, 2, 3, 4, 5], mats=2, W2ap=self.D['w3_%d' % l], K2=D)
                        S.barrier()
                with contextlib.ExitStack() as st2:
                    resid = self.make_resid(st2, xdst, xdst)

                    def epi_dn(b1, b2, col, nn, tc):
                        resid(self.ps[:, b1, :], [('ps', b1)], col // 128, half * 2 + tc)
                    self.linear_T(st2, self.D['w2_%d' % l], DFF, [(c, 128) for c in range(0, D, 128)], 128, gT, ['gT'], 2, epi_dn, [0, 1, 2, 3],
                                  cast=('pool', 'dve'))
                    S.barrier()
        if upto == 'C2':
            return
        with contextlib.ExitStack() as st:
            hT = self.sb(st, 'hTp', [128, 16, T], BF16)
            self.norm(st, xdst, 8 + l, hT, 0, T, 'hT')
            pT = self.sb(st, 'pTb', [128, 2, T], BF16)
            with contextlib.ExitStack() as st2:
                pf = self.sb(st2, 'pTf', [128, 2, T], F32)
                S.dma(pf[:], self.D['pT'][l].rearrange("c p t -> p c t"), [], ['pf'])
                S.op('dve', ['pf'], ['pT'], lambda v: v.tensor_copy(out=pT[:], in_=pf[:]))
                S.barrier()
            with contextlib.ExitStack() as st2:
                resid = self.make_resid(st2, xdst, xdst)
                sg = [self.sb(st2, 'sgp%d' % k, [128, 512], F32) for k in range(3)]
                cs = [0]

                def epi_g(b1, b2, col, nn, tc):
                    k = cs[0] % 3; cs[0] += 1
                    S.op('act', [('ps', b1)], [('sg', k)], lambda a: a.activation(out=sg[k][:], in_=self.ps[:, b1, :], func=AF.Sigmoid))
                    S.op('dve', [('sg', k), ('ps', b2)], [('sg', k)],
                         lambda v: v.tensor_tensor(out=sg[k][:], in0=sg[k][:], in1=self.ps[:, b2, :], op=ALU.mult))
                    resid(sg[k][:], [('sg', k)], col // 128, tc, eng='pool')
                self.linear_T(st2, self.D['w_pg_%d' % l], D, [(c, 256) for c in range(0, D, 256)], 256, hT, ['hT', 'pT'], 4, epi_g,
                              [0, 1, 2, 3, 4, 5], mats=2, W2ap=self.D['w_pe_%d' % l], K2=256, AT2=pT)
                S.barrier()

    def phase_S(self, xname, l, j, gname='G'):
        S = self.S
        d = self.dram
        qT = d('qT', [16, 128, T], BF16)
        aT = d('attnT', [16, 128, T], BF16)
        with contextlib.ExitStack() as st:
            hT = self.sb(st, 'hT', [128, 16, T], BF16)
            self.norm(st, xname, l, hT, 0, T, 'hT')
            with contextlib.ExitStack() as ph:
                evac = self.make_evac(ph)

                def epi(b1, b2, col, nn, tc):
                    evac(b1, nn, qT[col // 128, :, tc * 512:(tc + 1) * 512], [('qT', col // 128, tc)], mul=128.0 ** -0.5)
                self.linear_T(ph, self.D['w_q_b_%d' % j], D, [(c, 256) for c in range(0, D, 256)], 256, hT, ['hT'], 4, epi, [0, 1, 2, 3])
                S.barrier()
        with contextlib.ExitStack() as st:
            tb = self.bias_tables(st, False)
            es_ = self.sb(st, 'esink', [128, 32], F32)
            S.dma(es_[:], self.D['sinks'][:, :], [], ['esink'])
            S.op('act', ['esink'], ['esink'], lambda a: a.activation(out=es_[:], in_=es_[:], func=AF.Exp))
            kA = [self.sb(st, 'kA%d' % k, [128, 4, 4, 128], BF16) for k in range(2)]
            kB = [self.sb(st, 'kB%d' % k, [128, 4, 128], BF16) for k in range(2)]
            vA = [self.sb(st, 'vA%d' % k, [128, 4, 4, 128], BF16) for k in range(2)]
            vB = [self.sb(st, 'vB%d' % k, [128, 4, 128], BF16) for k in range(2)]
            qs = [self.sb(st, 'qs%d' % k, [128, 16, 128], BF16) for k in range(2)]
            PT = [self.sb(st, 'PT%d' % k, [128, 512], BF16) for k in range(4)]
            rd = [self.sb(st, 'rd%d' % k, [128, 512], F32) for k in range(2)]
            ao = [self.sb(st, 'ao%d' % k, [128, 512], BF16) for k in range(2)]
            cntP = [0]; sbi = [0]
            for i in range(16):
                sl = i % 2
                S.dma(qs[sl][:], qT[:, :, i * 128:(i + 1) * 128].rearrange("h d t -> d h t"), [('qT', h, i // 4) for h in range(16)], [('qs', sl)])
                for gg in range(4):
                    S.dma(kA[sl][:, :, gg, :], self.G3c(gname, gg)[:, :, i * 128:(i + 1) * 128], [], [('kA', sl, gg)])
                    S.dma(vA[sl][:, :, gg, :], self.G3c(gname, 4 + gg)[:, :, i * 128:(i + 1) * 128], [], [('vA', sl, gg)])
                    if i > 0:
                        S.dma(kB[sl][:, gg, :], self.G3c(gname, gg)[:, 3, (i - 1) * 128:i * 128], [], [('kB', sl, gg)])
                        S.dma(vB[sl][:, gg, :], self.G3c(gname, 4 + gg)[:, 3, (i - 1) * 128:i * 128], [], [('vB', sl, gg)])
                blocks = ([0] if i > 0 else []) + [1, 2, 3, 4]
                for g in range(4):
                    OB = 3 + (g % 2); DB = 5 + (g % 2)
                    for bi_, nb in enumerate(blocks):
                        b = sbi[0] % 3; sbi[0] += 1
                        kview = kB[sl][:, g, :] if nb == 0 else kA[sl][:, nb - 1, g, :]
                        vview = vB[sl][:, g, :] if nb == 0 else vA[sl][:, nb - 1, g, :]

                        def mm(p, b=b, kview=kview, nb=nb, g=g, sl=sl):
                            p.matmul(self.ps[:, b, :], lhsT=kview, rhs=qs[sl][:, g * 4:(g + 1) * 4, :], start=True, stop=False)
                            ins = None
                            for hh in range(4):
                                ins = p.matmul(self.ps[:, b, hh * 128:(hh + 1) * 128], lhsT=tb[:, g * 4 + hh, nb * 128:(nb + 1) * 128], rhs=self.ident[:],
                                               start=False, stop=(hh == 3))
                            return ins
                        S.op('pe', [('kA', sl, rr_) for rr_ in range(4)] + [('kB', sl, rr_) for rr_ in range(4)] + [('qs', sl), 'biastb', 'ident'], [('ps', b)], mm)
                        k = cntP[0] % 4; cntP[0] += 1
                        S.op('act', [('ps', b)], [('PT', k)], lambda a, b=b, k=k: a.activation(out=PT[k][:], in_=self.ps[:, b, :], func=AF.Exp))
                        first = (bi_ == 0); last = (bi_ == len(blocks) - 1)

                        def mm2(p, vview=vview, k=k, first=first, last=last, OB=OB, DB=DB):
                            p.matmul(self.ps[:, OB, :], lhsT=vview, rhs=PT[k][:], start=first, stop=last)
                            return p.matmul(self.ps[:, DB, :], lhsT=self.ones[:], rhs=PT[k][:], start=first, stop=last)
                        S.op('pe', [('vA', sl, rr_) for rr_ in range(4)] + [('vB', sl, rr_) for rr_ in range(4)] + [('PT', k), 'ones'], [('ps', OB), ('ps', DB)], mm2)
                    a = g % 2
                    for hh in range(4):
                        hcol = j * 16 + g * 4 + hh
                        S.op('dve', [('ps', DB), 'esink'], [('rd', a)],
                             lambda v, hh=hh, hcol=hcol, a=a, DB=DB: v.tensor_scalar(out=rd[a][:, hh * 128:(hh + 1) * 128], in0=self.ps[:, DB, hh * 128:(hh + 1) * 128],
                                                                                    scalar1=es_[:, hcol:hcol + 1], scalar2=None, op0=ALU.add))
                    S.op('dve', [('rd', a)], [('rd', a)], lambda v, a=a: v.reciprocal(out=rd[a][:], in_=rd[a][:]))
                    S.op('dve', [('ps', OB), ('rd', a)], [('ao', a)], lambda v, a=a, OB=OB: v.tensor_tensor(out=ao[a][:], in0=self.ps[:, OB, :], in1=rd[a][:], op=ALU.mult))
                    S.dma(aT[g * 4:(g + 1) * 4, :, i * 128:(i + 1) * 128].rearrange("h d t -> d h t"), ao[a][:].rearrange("p (h t) -> p h t", h=4),
                          [('ao', a)], [('attnT', g * 4 + hh, i // 4) for hh in range(4)])
            S.barrier()

    def gather(self, gname):
        S, nc = self.S, self.nc
        S.barrier()
        for k in range(9):
            L = self.Lc(k)
            G = self.dram('%s_%d' % (gname, k), [512, 2048], BF16)
            S.ccval += 1
            nc.gpsimd.collective_compute("AllGather", ALU.bypass, replica_groups=[[0, 1, 2, 3], [4, 5, 6, 7]],
                                         ins=[L.opt()], outs=[G.opt()]).then_inc(S.ccsem)
        for e in S.engs:
            S.engs[e].wait_ge(S.ccsem, S.ccval)

    def phase_final(self, xname):
        self.dram('yT', [16, 128, T], F32)
        with contextlib.ExitStack() as st:
            self.norm(st, xname, 13, None, 0, T, None, final='yT')


WNAMES = {'w_in_a': [2, D, 4176], 'w_out_a': [2, D, D], 'w_q_b': [2, D, D], 'w_out_b': [2, D, D], 'w_kv': [D, 1024],
          'w1': [4, D, DFF], 'w3': [4, D, DFF], 'w2': [4, DFF, D], 'w_pe': [4, 256, D], 'w_pg': [4, D, D]}
CONSTS = ['ident', 'gains', 'cmask', 'swamask', 'cb', 'biasg', 'sinks']
def _lw(l):
    return ['w1_%d' % l, 'w3_%d' % l, 'w2_%d' % l, 'w_pe_%d' % l, 'w_pg_%d' % l]


STAGE_INS = {1: ['xT_in', 'w_in_a_0'],
             2: ['xT_in', 'G', 'qT', 'qiT', 'wi', 'pT', 'w_in_a_1', 'w_out_a_0'] + _lw(0),
             3: ['xT_in', 'G', 'qT', 'qiT', 'wi', 'pT', 'w_out_a_1', 'w_kv'] + _lw(1),
             4: ['xT_in', 'G', 'pT', 'w_q_b_0', 'w_out_b_0', 'w_q_b_1', 'w_out_b_1'] + _lw(2) + _lw(3)}
STAGE_OUTS = {1: ['L', 'qT', 'qiT', 'wi'], 2: ['xT', 'L', 'qT2', 'qiT2', 'wi2'], 3: ['xT', 'L'], 4: ['yT']}


def build_fused():
    ins = ['xT_in', 'pT', 'w_in_a_0', 'w_in_a_1', 'w_out_a_0', 'w_out_a_1', 'w_kv', 'w_q_b_0', 'w_out_b_0', 'w_q_b_1', 'w_out_b_1']
    for l in range(4):
        ins += _lw(l)
    P = Prog(ins + CONSTS, ['yT'])
    d = P.dram
    d('xT_in', [16, 128, T], F32)
    d('pT', [4, 2, 128, T], F32)
    for w in ins:
        if w == 'w_kv':
            d(w, WNAMES[w], F32)
        elif w.rsplit('_', 1)[0] in WNAMES:
            d(w, WNAMES[w.rsplit('_', 1)[0]][1:], F32)
    es = contextlib.ExitStack()
    with es:
        P.begin(es)
        d('xT', [16, 128, T], F32)
        P.phase_A('xT_in', 0)
        P.gather('G0')
        P.phase_B('G0')
        P.phase_C('xT_in', 'xT', 0, 'w_out_a', 0)
        P.phase_A('xT', 1)
        P.gather('G1')
        P.phase_B('G1')
        P.phase_C('xT', 'xT', 1, 'w_out_a', 1)
        P.phase_KV('xT')
        P.gather('G0')
        P.phase_S('xT', 2, 0, 'G0')
        P.phase_C('xT', 'xT', 2, 'w_out_b', 0)
        P.phase_S('xT', 3, 1, 'G0')
        P.phase_C('xT', 'xT', 3, 'w_out_b', 1)
        P.phase_final('xT')
        P.S.finish()
    return P.nc, ins


def t5_bucket_np(rel):
    n = np.maximum(rel, 0)
    nf = np.maximum(n, 1).astype(np.float32)
    large = 16 + (np.log(nf / np.float32(16)) / np.float32(math.log(128 / 16)) * np.float32(16)).astype(np.int32)
    large = np.minimum(large, 31)
    return np.where(n < 16, n, large)


def host_consts(inputs, r):
    rel_bias = np.asarray(inputs['rel_bias'], np.float32)
    t = np.arange(128)[:, None]
    s = np.arange(128)[None, :]
    Ds = [r + 1] + [r - rp for rp in range(4)]
    cmask = np.zeros((128, 4, 128), np.float32)
    for rp in range(4):
        if rp > r:
            cmask[:, rp, :] = NEG
        elif rp == r:
            cmask[:, rp, :] = np.where(s <= t, 0.0, NEG)
    swam = np.zeros((128, 5, 128), np.float32)
    biasg = np.zeros((128, 16, 5, 128), np.float32)
    for bi, Dk in enumerate(Ds):
        rel = t - s + 128 * Dk
        ok = (rel >= 0) & (rel < 128)
        swam[:, bi, :] = np.where(ok, 0.0, MNEG)
        bidx = t5_bucket_np(rel) if Dk >= 0 else np.full((128, 128), 31)
        biasg[:, :, bi, :] = rel_bias[bidx].transpose(0, 2, 1)
    gl = [inputs['g_attn'][i] for i in range(4)] + [inputs['g_ffn'][i] for i in range(4)] + [inputs['g_pe'][i] for i in range(4)] \
        + [inputs['g_kv'], inputs['g_final']]
    gains = np.stack([np.asarray(g, np.float32) for g in gl]).reshape(14, 16, 128).transpose(2, 0, 1).reshape(128, 224)
    return {
        'ident': np.eye(128, dtype=np.float32),
        'gains': np.ascontiguousarray(gains),
        'cmask': np.ascontiguousarray(cmask.reshape(128, 512)),
        'swamask': np.ascontiguousarray(swam.reshape(128, 640)),
        'cb': np.ascontiguousarray(np.broadcast_to(rel_bias[31][None, :], (128, 16))),
        'biasg': np.ascontiguousarray(biasg.reshape(128, 16 * 640)),
        'sinks': np.ascontiguousarray(np.broadcast_to(np.asarray(inputs['sinks'], np.float32).reshape(1, 32), (128, 32))),
    }


def host_prep(inputs):
    x = np.asarray(inputs['x'], np.float32)
    p = np.asarray(inputs['p'], np.float32)
    W = {k: np.ascontiguousarray(np.asarray(inputs[k], np.float32)) for k in WNAMES}
    cst, xT, pT = [], [], []
    for c in range(8):
        b, r = c // 4, c % 4
        cst.append(host_consts(inputs, r))
        xc = x[b].reshape(16, 4, 128, D)[:, r].reshape(T, D)
        xT.append(np.ascontiguousarray(xc.T).reshape(16, 128, T))
        pc = p[:, b].reshape(4, 16, 4, 128, 256)[:, :, r].reshape(4, T, 256)
        pT.append(np.ascontiguousarray(pc.transpose(0, 2, 1)).reshape(4, 2, 128, T))
    return W, cst, xT, pT


def kernel(**inputs):
    W, cst, xT, pT = host_prep(inputs)
    nc, ins = build_fused()
    wts = {}
    for w in ins:
        if w == 'w_kv':
            wts[w] = W[w]
        elif w.rsplit('_', 1)[0] in WNAMES:
            wts[w] = np.ascontiguousarray(W[w.rsplit('_', 1)[0]][int(w.rsplit('_', 1)[1])])
    in_maps = []
    for c in range(8):
        m = dict(cst[c], xT_in=xT[c], pT=pT[c])
        m.update(wts)
        in_maps.append(m)
    res = run_bass_kernel_spmd(nc, in_maps, core_ids=list(range(8))).results
    out = np.zeros((2, 8192, D), np.float32)
    for c in range(8):
        b, r = c // 4, c % 4
        yc = res[c]['yT'].reshape(D, T).T
        out[b].reshape(16, 4, 128, D)[:, r] = yc.reshape(16, 128, D)
    return out
```
